# Optimizing a Trainium2 kernel written in Bass

```python
import math
import jax, jax.numpy as jnp
from jax import lax
import numpy as np

D_MODEL = 2048
BATCH = 2
SEQ = 16384
DEPTH = 2

N_MEM = 256
HEAD_DIM = 128
MOBA_HEADS = 8
MOBA_BLOCK = 256
MOBA_TOPK = 3
MOBA_QBLOCK = 128
HGRN_HEADS = 4
HGRN_DK = 128
HGRN_DV = 128
HGRN_CHUNK = 64
MEM_HEADS = 4
D_FF = 5632
N_BRANCH = 3
RMS_EPS = 1e-6
NEG_INF = -1e30
F_MIN = 1e-20

MOBA_W = MOBA_HEADS * HEAD_DIM
HGRN_KW = HGRN_HEADS * HGRN_DK
HGRN_VW = HGRN_HEADS * HGRN_DV
MEM_W = MEM_HEADS * HEAD_DIM
N_IN = 3 * MOBA_W + 2 * HGRN_KW + 2 * HGRN_VW + MEM_W + N_BRANCH * D_MODEL

kernel_name = "hybrid_moba_hgrn2_memory_macaron"


def rms_norm(x, g):
    xf = x.astype(jnp.float32)
    r = lax.rsqrt(jnp.mean(xf * xf, axis=-1, keepdims=True) + RMS_EPS)
    return (xf * r).astype(x.dtype) * g


def swiglu_ffn(h, w1, w3, w2):
    return (jax.nn.silu(h @ w1) * (h @ w3)) @ w2


def alibi_slopes(n):
    return jnp.exp2(-8.0 * jnp.arange(1, n + 1, dtype=jnp.float32) / n)


def split_columns(p):
    sizes = (MOBA_W, MOBA_W, MOBA_W, HGRN_KW, HGRN_KW, HGRN_VW, HGRN_VW, MEM_W, N_BRANCH * D_MODEL)
    outs, off = [], 0
    for s in sizes:
        outs.append(p[..., off:off + s])
        off += s
    return outs


def moba_attention(q, k, v):
    B, T, H, Dh = q.shape
    BS, QB = MOBA_BLOCK, MOBA_QBLOCK
    Tp = -(-T // BS) * BS
    NB, NQ = Tp // BS, Tp // QB
    K = min(MOBA_TOPK, NB)
    scale = Dh ** -0.5

    def heads_major(a):
        a = jnp.pad(a, ((0, 0), (0, Tp - T), (0, 0), (0, 0)))
        return a.transpose(0, 2, 1, 3).reshape(B * H, Tp, Dh)

    qh, kh, vh = heads_major(q), heads_major(k), heads_major(v)
    kb = kh.reshape(B * H, NB, BS, Dh)
    vb = vh.reshape(B * H, NB, BS, Dh)
    k_mean = jnp.mean(kb.astype(jnp.float32), axis=2)
    t = jnp.arange(Tp, dtype=jnp.int32)
    n_past = t // BS
    past = jnp.arange(NB, dtype=jnp.int32)[None, :] < n_past[:, None]
    gate = jnp.einsum('ntd,nbd->ntb', qh.astype(jnp.float32), k_mean)
    gate = jnp.where(past[None], gate, NEG_INF)
    _, sel = lax.top_k(gate, K)
    sel_ok = (jnp.arange(K, dtype=jnp.int32)[None, :] < n_past[:, None]).reshape(NQ, QB, K)
    slopes = jnp.tile(alibi_slopes(H), B)
    offs_s = jnp.arange(BS, dtype=jnp.int32)
    offs_q = jnp.arange(QB, dtype=jnp.int32)

    def one_head(args):
        q_n, k_n, v_n, sel_n, m = args

        def one_block(args2):
            c, q_c, sel_c, ok_c = args2
            t_q = c * QB + offs_q
            own = (c * QB) // BS
            k_own, v_own = k_n[own], v_n[own]
            k_sel, v_sel = k_n[sel_c], v_n[sel_c]
            pos_own = own * BS + offs_s
            pos_sel = sel_c[..., None] * BS + offs_s
            d_own = (t_q[:, None] - pos_own[None, :]).astype(jnp.float32)
            d_sel = (t_q[:, None, None] - pos_sel).astype(jnp.float32)
            l_own = jnp.einsum('qd,sd->qs', q_c, k_own).astype(jnp.float32) * scale - m * d_own
            l_own = jnp.where(d_own >= 0, l_own, NEG_INF)
            l_sel = jnp.einsum('qd,qksd->qks', q_c, k_sel).astype(jnp.float32) * scale - m * d_sel
            l_sel = jnp.where(ok_c[..., None], l_sel, NEG_INF)
            p = jax.nn.softmax(jnp.concatenate([l_sel.reshape(QB, K * BS), l_own], axis=-1), axis=-1)
            p_sel = p[:, :K * BS].reshape(QB, K, BS).astype(v_n.dtype)
            p_own = p[:, K * BS:].astype(v_n.dtype)
            return jnp.einsum('qks,qksd->qd', p_sel, v_sel) + jnp.einsum('qs,sd->qd', p_own, v_own)

        return lax.map(one_block, (jnp.arange(NQ, dtype=jnp.int32), q_n, sel_n, sel_ok))

    out = lax.map(one_head, (qh.reshape(B * H, NQ, QB, Dh), kb, vb,
                             sel.reshape(B * H, NQ, QB, K), slopes))
    return out.reshape(B, H, Tp, Dh).transpose(0, 2, 1, 3)[:, :T]


def hgrn2_chunkwise(q, k, v, log_f):
    B, T, H, Dk = q.shape
    Dv = v.shape[-1]
    C = HGRN_CHUNK
    NC = T // C

    def to_chunks(a):
        return a.astype(jnp.float32).reshape(B, NC, C, H, a.shape[-1]).transpose(1, 0, 3, 2, 4)

    qc, kc, vc, gc = to_chunks(q), to_chunks(k), to_chunks(v), to_chunks(log_f)
    causal = jnp.tril(jnp.ones((C, C), dtype=bool))[:, :, None]

    def step(S, xs):
        qi, ki, vi, gi = xs
        G = jnp.cumsum(gi, axis=2)
        o_inter = jnp.einsum('bhcd,bhdv->bhcv', qi * jnp.exp(G), S)
        diff = G[:, :, :, None, :] - G[:, :, None, :, :]
        decay = jnp.where(causal, jnp.exp(jnp.minimum(diff, 0.0)), 0.0)
        A = jnp.einsum('bhtsd,bhsd->bhts', qi[:, :, :, None, :] * decay, ki)
        o_intra = jnp.einsum('bhts,bhsv->bhtv', A, vi)
        G_last = G[:, :, -1]
        S_new = jnp.exp(G_last)[..., None] * S + jnp.einsum(
            'bhsd,bhsv->bhdv', ki * jnp.exp(G_last[:, :, None] - G), vi)
        return S_new, o_inter + o_intra

    S0 = jnp.zeros((B, H, Dk, Dv), jnp.float32)
    _, o = lax.scan(step, S0, (qc, kc, vc, gc))
    return o.transpose(1, 0, 3, 2, 4).reshape(B, T, H, Dv)


def memory_attention(q, mk, mv):
    s = jnp.einsum('bthd,bmhd->bhtm', q, mk).astype(jnp.float32) * (q.shape[-1] ** -0.5)
    p = jax.nn.softmax(s, axis=-1).astype(mv.dtype)
    return jnp.einsum('bhtm,bmhd->bthd', p, mv)


def setup_inputs(seed: int = 0) -> dict:
    key = jax.random.key(seed)
    ks = jax.random.split(key, 24)
    f32 = jnp.float32

    def dense(k, shape, fan_in):
        return jax.random.normal(k, shape, f32) * (fan_in ** -0.5)

    def gain(k, shape):
        return 1.0 + 0.02 * jax.random.normal(k, shape, f32)

    L, D = DEPTH, D_MODEL
    return {
        'x': jax.random.normal(ks[0], (BATCH, SEQ, D), f32),
        'mem': jax.random.normal(ks[1], (BATCH, N_MEM, D), f32),
        'ffn1_norm': gain(ks[2], (L, D)),
        'ffn1_w1': dense(ks[3], (L, D, D_FF), D),
        'ffn1_w3': dense(ks[4], (L, D, D_FF), D),
        'ffn1_w2': dense(ks[5], (L, D_FF, D), D_FF),
        'mix_norm': gain(ks[6], (L, D)),
        'w_in': dense(ks[7], (L, D, N_IN), D),
        'hgrn_lb_logits': 0.5 * jax.random.normal(ks[8], (L, HGRN_KW), f32),
        'hgrn_out_norm': gain(ks[9], (L, HGRN_DV)),
        'mem_norm': gain(ks[10], (L, D)),
        'w_mem_kv': dense(ks[11], (L, D, 2 * MEM_W), D),
        'w_proj_moba': dense(ks[12], (L, MOBA_W, D), MOBA_W),
        'w_proj_hgrn': dense(ks[13], (L, HGRN_VW, D), HGRN_VW),
        'w_proj_mem': dense(ks[14], (L, MEM_W, D), MEM_W),
        'w_out': dense(ks[15], (L, D, D), D),
        'ffn2_norm': gain(ks[16], (L, D)),
        'ffn2_w1': dense(ks[17], (L, D, D_FF), D),
        'ffn2_w3': dense(ks[18], (L, D, D_FF), D),
        'ffn2_w2': dense(ks[19], (L, D_FF, D), D_FF),
        'final_norm': gain(ks[20], (D,)),
    }


def reference(x, mem, ffn1_norm, ffn1_w1, ffn1_w3, ffn1_w2, mix_norm, w_in, hgrn_lb_logits,
              hgrn_out_norm, mem_norm, w_mem_kv, w_proj_moba, w_proj_hgrn, w_proj_mem, w_out,
              ffn2_norm, ffn2_w1, ffn2_w3, ffn2_w2, final_norm):
    B, T, _ = x.shape
    M = mem.shape[1]
    p_lb = jax.nn.softmax(hgrn_lb_logits.astype(jnp.float32), axis=0)
    lower_bounds = jnp.cumsum(p_lb, axis=0) - p_lb[0:1]

    for l in range(DEPTH):
        x = x + 0.5 * swiglu_ffn(rms_norm(x, ffn1_norm[l]), ffn1_w1[l], ffn1_w3[l], ffn1_w2[l])

        h = rms_norm(x, mix_norm[l])
        qa, ka, va, qb, fb, ib, gb, qm, gates = split_columns(h @ w_in[l])

        hd = (B, T, MOBA_HEADS, HEAD_DIM)
        oa = moba_attention(qa.reshape(hd), ka.reshape(hd), va.reshape(hd)).reshape(B, T, MOBA_W)

        lb = lower_bounds[l]
        fb32 = fb.astype(jnp.float32)
        f_gate = lb + (1.0 - lb) * jax.nn.sigmoid(fb32)
        log_f = jnp.log(jnp.maximum(f_gate, F_MIN))
        k_b = (1.0 - lb) * jax.nn.sigmoid(-fb32)
        kd = (B, T, HGRN_HEADS, HGRN_DK)
        vd = (B, T, HGRN_HEADS, HGRN_DV)
        ob = hgrn2_chunkwise(jax.nn.silu(qb).reshape(kd), k_b.reshape(kd), ib.reshape(vd),
                             log_f.reshape(kd)).astype(x.dtype)
        ob = (rms_norm(ob, hgrn_out_norm[l]) * jax.nn.sigmoid(gb.reshape(vd))).reshape(B, T, HGRN_VW)

        kv = rms_norm(mem, mem_norm[l]) @ w_mem_kv[l]
        md = (B, M, MEM_HEADS, HEAD_DIM)
        om = memory_attention(qm.reshape(B, T, MEM_HEADS, HEAD_DIM),
                              kv[..., :MEM_W].reshape(md), kv[..., MEM_W:].reshape(md)).reshape(B, T, MEM_W)

        g = jax.nn.sigmoid(gates).reshape(B, T, N_BRANCH, D_MODEL)
        y = (g[:, :, 0] * (oa @ w_proj_moba[l])
             + g[:, :, 1] * (ob @ w_proj_hgrn[l])
             + g[:, :, 2] * (om @ w_proj_mem[l]))
        x = x + y @ w_out[l]

        x = x + 0.5 * swiglu_ffn(rms_norm(x, ffn2_norm[l]), ffn2_w1[l], ffn2_w3[l], ffn2_w2[l])

    return rms_norm(x, final_norm)
```

```python
import contextlib
import numpy as np
import concourse.bass as bass
import concourse.mybir as mybir
from concourse.bass_utils import run_bass_kernel_spmd

F32 = mybir.dt.float32
BF16 = mybir.dt.bfloat16
AF = mybir.ActivationFunctionType
ALU = mybir.AluOpType
AX = mybir.AxisListType

SEM_LIMIT = 30000


class Buf:
    __slots__ = ("name", "last_w", "readers", "sem", "dcnt", "multi", "last_dma", "excl")

    def __init__(self, name, multi=False):
        self.name = name
        self.last_w = None
        self.readers = {}
        self.sem = None
        self.dcnt = 0
        self.multi = multi
        self.last_dma = None
        self.excl = False


class Op:
    __slots__ = ("eng", "fn", "deps", "dma", "flag", "sem", "val", "dbuf", "n")

    def __init__(self, eng, fn, dma):
        self.eng = eng
        self.fn = fn
        self.dma = dma
        self.deps = []
        self.flag = False
        self.sem = None
        self.val = 0
        self.dbuf = None
        self.n = 0


class Prog:
    ENGS = ("pe", "act", "dve", "pool", "sp")

    _count = 0

    def __init__(self, nc):
        self.nc = nc
        Prog._count += 1
        self.pid = Prog._count
        self.ops = {e: [] for e in self.ENGS}
        self.stack = contextlib.ExitStack()
        self.dma_bufs = []
        self.nops = 0

    def sb(self, name, shape, dt):
        return self.stack.enter_context(self.nc.sbuf_tensor(f"sb{self.pid}_" + name, list(shape), dt))

    def ps(self, name, shape, dt=F32):
        return self.stack.enter_context(self.nc.psum_tensor(f"ps{self.pid}_" + name, list(shape), dt))

    def add(self, eng, fn, r=(), w=(), dma=False, semb=None):
        import os
        if self.nops >= int(os.environ.get("MAXOPS", "100000000")):
            return None
        op = Op(eng, fn, dma)
        self.nops += 1
        op.n = self.nops
        deps = {}

        def dep(p, raw):
            if p is None or p is op:
                return
            if not p.dma and p.eng == eng:
                if eng == "pe" and not dma:
                    return
                if not raw and not dma:
                    return
            deps[id(p)] = p

        for b in r:
            dep(b.last_w, True)
            if b.excl:
                for k_, rd in b.readers.items():
                    if k_ != eng:
                        dep(rd, True)
        for b in w:
            if not b.multi:
                dep(b.last_w, False)
            for rd in b.readers.values():
                dep(rd, False)
        for b in r:
            key = ("dma", id(op)) if dma else eng
            if dma:
                b.readers[("dma", id(semb if semb is not None else w[0]))] = op
            else:
                b.readers[key] = op
        for b in w:
            b.last_w = op
            b.readers = {}
        if dma:
            db = semb if semb is not None else w[0]
            op.dbuf = db
            if db.sem is None:
                db.sem = "pending"
                self.dma_bufs.append(db)
            db.dcnt += 1
            op.val = 16 * db.dcnt
            db.last_dma = op
        for p in deps.values():
            if not p.dma:
                p.flag = True
        op.deps = list(deps.values())
        self.ops[eng].append(op)
        return op

    def barrier(self, bufs):
        lasts = []
        for e in self.ENGS:
            for p in reversed(self.ops[e]):
                if not p.dma:
                    lasts.append(p)
                    break
        dl = []
        for b in self.dma_bufs:
            if b.last_w is not None and b.last_w.dma:
                dl.append(b.last_w)
            for rd in b.readers.values():
                if rd.dma:
                    dl.append(rd)
        for b in bufs:
            for rd in b.readers.values():
                if rd.dma:
                    dl.append(rd)
            if b.last_w is not None and b.last_w.dma:
                dl.append(b.last_w)
        for e in self.ENGS:
            op = Op(e, None, False)
            self.nops += 1
            for p in lasts:
                if p.eng != e:
                    p.flag = True
                    op.deps.append(p)
            op.deps.extend(dl)
            self.ops[e].append(op)

    def finish(self, out_bufs=None):
        op = Op("sp", None, False)
        for b in self.dma_bufs:
            if b.last_dma is not None:
                op.deps.append(b.last_dma)
        self.ops["sp"].append(op)

    def emit(self):
        nc = self.nc
        st = self.stack
        eng_sems = {}
        for e in self.ENGS:
            n = 0
            for op in self.ops[e]:
                if op.flag and not op.dma:
                    n += 1
            nsem = (n + SEM_LIMIT - 1) // SEM_LIMIT
            eng_sems[e] = [st.enter_context(nc.semaphore(f"s{self.pid}_{e}{i}")) for i in range(nsem)]
            c = 0
            for op in self.ops[e]:
                if op.flag and not op.dma:
                    op.sem = eng_sems[e][c // SEM_LIMIT]
                    op.val = c % SEM_LIMIT + 1
                    c += 1
        for i, b in enumerate(self.dma_bufs):
            b.sem = st.enter_context(nc.semaphore(f"d{self.pid}_{i}_{b.name}"))
        nsem_total = sum(len(v) for v in eng_sems.values()) + len(self.dma_bufs)
        self.nsem = nsem_total

        def run(e, engobj):
            waited = {}
            for op in self.ops[e]:
                need = {}
                for p in op.deps:
                    sem = p.dbuf.sem if p.dma else p.sem
                    k = id(sem)
                    if k not in need or need[k][1] < p.val:
                        need[k] = (sem, p.val)
                for k, (sem, val) in need.items():
                    if waited.get(k, 0) >= val:
                        continue
                    engobj.wait_ge(sem, val)
                    waited[k] = val
                if op.fn is None:
                    continue
                ins = op.fn(engobj)
                if op.dma:
                    ins.then_inc(op.dbuf.sem, 16)
                elif op.flag:
                    ins.then_inc(op.sem, 1)

        with nc.Block() as block:
            @block.tensor
            def _(eng):
                run("pe", eng)

            @block.scalar
            def _(eng):
                run("act", eng)

            @block.vector
            def _(eng):
                run("dve", eng)

            @block.gpsimd
            def _(eng):
                run("pool", eng)

            @block.sync
            def _(eng):
                run("sp", eng)
        nc.all_engine_barrier()
        st.close()


class Rot:
    def __init__(self, P, name, n, shape, dt, psum=False):
        self.t = []
        self.b = []
        for i in range(n):
            t = P.ps(f"{name}{i}", shape, dt) if psum else P.sb(f"{name}{i}", shape, dt)
            self.t.append(t)
            self.b.append(Buf(f"{name}{i}"))
            self.b[-1].excl = psum
        self.i = -1
        self.n = n

    def next(self):
        self.i = (self.i + 1) % self.n
        return self.t[self.i], self.b[self.i]


D = 2048
KD = 16
DFF = 5632
NF = 44
TT = 512
EPS = 1e-6
FMIN = 1e-20
SCALE = 128 ** -0.5
NEG = -32768.0
G_FFN1, G_MIX, G_FFN2, G_MEM, G_FIN = 0, 1, 2, 3, 4


class Ctx:
    pass


def mm(P, out, lhsT, rhs, start, stop, r, w, **kw):
    P.add("pe", lambda e: e.matmul(out, lhsT=lhsT, rhs=rhs, start=start, stop=stop, **kw), r=r, w=w)


def tp(P, out, in_, ident, r, w):
    P.add("pe", lambda e: e.transpose(out, in_, ident), r=r, w=w)


def act(P, out, in_, func, r, w, bias=None, scale=None):
    kw = {}
    if bias is not None:
        kw["bias"] = bias
    if scale is not None:
        kw["scale"] = scale
    P.add("act", lambda e: e.activation(out=out, in_=in_, func=func, **kw), r=r, w=w)


def tt(P, eng, out, in0, in1, op, r, w):
    P.add(eng, lambda e: e.tensor_tensor(out=out, in0=in0, in1=in1, op=op), r=r, w=w)


def ts(P, eng, out, in0, s1, s2, op0, op1, r, w):
    if s2 is None:
        P.add(eng, lambda e: e.tensor_scalar(out=out, in0=in0, scalar1=s1, scalar2=None, op0=op0), r=r, w=w)
    else:
        P.add(eng, lambda e: e.tensor_scalar(out=out, in0=in0, scalar1=s1, scalar2=s2, op0=op0, op1=op1), r=r, w=w)


def stt(P, out, in0, scalar, in1, op0, op1, r, w):
    P.add("dve", lambda e: e.scalar_tensor_tensor(out=out, in0=in0, scalar=scalar, in1=in1, op0=op0, op1=op1), r=r, w=w)


def cp(P, eng, out, in_, r, w):
    if eng == "act":
        P.add("act", lambda e: e.copy(out=out, in_=in_), r=r, w=w)
    else:
        P.add(eng, lambda e: e.tensor_copy(out=out, in_=in_), r=r, w=w)


def dma(P, q, out, in_, r, w):
    store = str(out.space).endswith("DRAM")
    P.add(q, lambda e: e.dma_start(out=out, in_=in_), r=r, w=w, dma=True, semb=(r[0] if store else w[0]))


def end_phase(P, nc):
    P.finish(None)
    P.emit()
    nc.all_engine_barrier()


def setup_common(P, C):
    C.onesD = P.sb("onesD", [128, 128], BF16)
    C.ones1 = P.sb("ones1", [128, 128], BF16)
    C.cb = Buf("consts")
    P.add("pool", lambda e: e.memset(C.onesD[:, :], 1.0 / D), w=[C.cb])
    P.add("pool", lambda e: e.memset(C.ones1[:, :], 1.0), w=[C.cb])
    C.ro = Buf("ro")


def phase_cast(nc, jobs):
    P = Prog(nc)
    ro = Buf("ro")
    CH = 4096
    s32 = Rot(P, "c32", 3, [128, CH], F32)
    s16 = Rot(P, "c16", 3, [128, CH], BF16)
    i = 0
    for (src, dst, n, F) in jobs:
        db = Buf("wdst", multi=True)
        for j in range(n):
            for c0 in range(0, F, CH):
                cw = min(CH, F - c0)
                t32, b32 = s32.next()
                t16, b16 = s16.next()
                dma(P, "sp", t32[:, 0:cw], src[j, :, c0:c0 + cw], [ro], [b32])
                eng = ("act", "dve", "pool", "act", "dve")[i % 5]
                i += 1
                cp(P, eng, t16[:, 0:cw], t32[:, 0:cw], [b32], [b16])
                dma(P, "pool" if eng != "pool" else "sp", dst[j, :, c0:c0 + cw], t16[:, 0:cw], [b16], [db])
    end_phase(P, nc)


def rmsnorm_T(P, C, x3, xb, gT, gTb, gcol, out3, outb, sq3, sqb, pbank, rstd, rstdb, Tt):
    pt, pb = pbank
    half = KD // 2
    P.add("act", lambda e: e.activation(out=sq3[:, 0:half, :], in_=x3[:, 0:half, :], func=AF.Square), r=[xb], w=[sqb])
    tt(P, "pool", sq3[:, half:KD, :], x3[:, half:KD, :], x3[:, half:KD, :], ALU.mult, [xb], [sqb])
    for k in range(KD):
        mm(P, pt[:, 0:Tt], C.onesD[:, :], sq3[:, k, :], k == 0, k == KD - 1, [sqb, C.cb], [pb])
    ts(P, "dve", rstd[:, 0:Tt], pt[:, 0:Tt], EPS, None, ALU.add, None, [pb], [rstdb])
    act(P, rstd[:, 0:Tt], rstd[:, 0:Tt], AF.Sqrt, [rstdb], [rstdb])
    P.add("dve", lambda e: e.reciprocal(out=rstd[:, 0:Tt], in_=rstd[:, 0:Tt]), r=[rstdb], w=[rstdb])
    for k in range(KD):
        stt(P, out3[:, k, :], x3[:, k, :], gT[:, gcol * KD + k:gcol * KD + k + 1], rstd[:, 0:Tt], ALU.mult, ALU.mult,
            [xb, rstdb, gTb], [outb])


class FFNRes:
    def __init__(self, P):
        self.g = P.sb("g", [128, NF, TT], BF16)
        self.gb = Buf("g")
        self.w13 = Rot(P, "w13", 4, [128, KD * 128], BF16)
        self.w2 = Rot(P, "w2", 2, [128, NF * 128], BF16)
        self.pu = Rot(P, "pu", 2, [128, 512], F32, psum=True)
        self.pv = Rot(P, "pv", 2, [128, 512], F32, psum=True)
        self.py = Rot(P, "py", 2, [128, 512], F32, psum=True)
        self.s = Rot(P, "s", 2, [128, TT], F32)


def ffn_tile(P, C, F, xt, xb, hT, hb, W1, W3, W2):
    Tt = TT
    g, gb = F.g, F.gb
    for j in range(NF):
        w1, w1b = F.w13.next()
        dma(P, "sp", w1[:, :], W1[j, :, :], [C.ro], [w1b])
        w3, w3b = F.w13.next()
        dma(P, "sp", w3[:, :], W3[j, :, :], [C.ro], [w3b])
        pu, pub = F.pu.next()
        pv, pvb = F.pv.next()
        for k in range(KD):
            mm(P, pu[:, 0:Tt], w1[:, k * 128:(k + 1) * 128], hT[:, k, :], k == 0, k == KD - 1, [w1b, hb], [pub])
        for k in range(KD):
            mm(P, pv[:, 0:Tt], w3[:, k * 128:(k + 1) * 128], hT[:, k, :], k == 0, k == KD - 1, [w3b, hb], [pvb])
        s, sbb = F.s.next()
        act(P, s[:, :], pu[:, 0:Tt], AF.Silu, [pub], [sbb])
        tt(P, "dve", g[:, j, :], s[:, :], pv[:, 0:Tt], ALU.mult, [sbb, pvb], [gb])
    for o in range(KD):
        w2, w2b = F.w2.next()
        dma(P, "sp", w2[:, :], W2[o, :, :], [C.ro], [w2b])
        py, pyb = F.py.next()
        for fk in range(NF):
            mm(P, py[:, 0:Tt], w2[:, fk * 128:(fk + 1) * 128], g[:, fk, :], fk == 0, fk == NF - 1, [w2b, gb], [pyb])
        stt(P, xt[:, o, :], py[:, 0:Tt], 0.5, xt[:, o, :], ALU.mult, ALU.add, [pyb, xb], [xb])


def phase_ffn(nc, NT, xin, xout, gains_d, gcol, W1, W3, W2, final_gcol=None):
    P = Prog(nc)
    C = Ctx()
    setup_common(P, C)
    gT = P.sb("gT", [128, 5 * KD], F32)
    gTb = Buf("gT")
    dma(P, "sp", gT[:, :], gains_d[:, :], [C.ro], [gTb])
    F = FFNRes(P)
    xts = Rot(P, "xt", 2, [128, KD, TT], F32)
    hT = P.sb("hT", [128, KD, TT], BF16)
    hb = Buf("hT")
    rstd = P.sb("rstd", [128, TT], F32)
    rstdb = Buf("rstd")
    pss = Rot(P, "pss", 1, [128, 512], F32, psum=True)
    ob = Buf("xout", multi=True)
    sq3 = F.g[:, 0:KD, :]
    for i in range(NT):
        xt, xb = xts.next()
        dma(P, "sp", xt[:, :, :], xin[i].rearrange("p (k t) -> p k t", k=KD), [C.ro], [xb])
        rmsnorm_T(P, C, xt, xb, gT, gTb, gcol, hT, hb, sq3, F.gb, pss.next(), rstd, rstdb, TT)
        ffn_tile(P, C, F, xt, xb, hT, hb, W1, W3, W2)
        if final_gcol is not None:
            rmsnorm_T(P, C, xt, xb, gT, gTb, final_gcol, xt, xb, sq3, F.gb, pss.next(), rstd, rstdb, TT)
        dma(P, "pool", xout[i].rearrange("p (k t) -> p k t", k=KD), xt[:, :, :], [xb], [ob])
    end_phase(P, nc)


def phase_m1(nc, NT, io):
    P = Prog(nc)
    C = Ctx()
    setup_common(P, C)
    ro = C.ro
    TC = NT * TT
    NBc = TC // 256
    gT = P.sb("gT", [128, 5 * KD], F32); gTb = Buf("gT")
    dma(P, "sp", gT[:, :], io["gains"][:, :], [ro], [gTb])
    ident = P.sb("ident", [128, 128], BF16)
    tri = P.sb("tri", [64, 64], BF16)
    smask = P.sb("smask", [128, TT], F32)
    lbl = P.sb("lbl", [128, 16], F32)
    lsel = P.sb("lsel", [128, 16], F32)
    cb2 = Buf("c2")
    dma(P, "sp", ident[:, :], io["ident"][:, :], [ro], [cb2])
    dma(P, "sp", tri[:, :], io["tri64"][:, :], [ro], [cb2])
    dma(P, "sp", smask[:, :], io["scanmask"][:, :], [ro], [cb2])
    dma(P, "sp", lbl[:, :], io["lbT"][:, :], [ro], [cb2])
    dma(P, "sp", lsel[:, :], io["lsel"][:, :], [ro], [cb2])
    lb = P.sb("lb", [128, 4], F32)
    oml = P.sb("oml", [128, 4], F32)
    lbm1 = P.sb("lbm1", [128, 4], F32)
    lbb = Buf("lb")
    tt(P, "dve", lb[:, :], lbl[:, 4:8], lbl[:, 0:4], ALU.subtract, [cb2], [lbb])
    act(P, lb[:, :], lb[:, :], AF.Sigmoid, [lbb], [lbb])
    ts(P, "dve", lb[:, :], lb[:, :], lsel[:, 0:1], None, ALU.mult, None, [lbb, cb2], [lbb])
    ts(P, "dve", lbm1[:, :], lb[:, :], -1.0, None, ALU.add, None, [lbb], [lbb])
    ts(P, "dve", oml[:, :], lbm1[:, :], -1.0, None, ALU.mult, None, [lbb], [lbb])

    xt = P.sb("xt", [128, KD, TT], F32); xb = Buf("xt")
    hT = P.sb("hT", [128, KD, TT], BF16); hb = Buf("hT")
    scrA = P.sb("scrA", [128, KD, TT], BF16); scrb = Buf("scrA")
    rstd = P.sb("rstd", [128, TT], F32); rstdb = Buf("rstd")
    w13 = Rot(P, "w13", 3, [128, KD * 128], BF16)
    wT = Rot(P, "wT", 2, [128, KD * 256], BF16)
    pp = Rot(P, "pp", 2, [128, 512], F32, psum=True)
    pq = Rot(P, "pq", 2, [128, 512], F32, psum=True)
    pU = Rot(P, "pU", 1, [128, 512], F32, psum=True)
    pAT = Rot(P, "pAT", 1, [128, 512], F32, psum=True)
    pO = Rot(P, "pO", 1, [128, 512], F32, psum=True)
    pTr = Rot(P, "pTr", 1, [128, 1024], BF16, psum=True)

    def projF(ci, h3, Tt):
        w, wb_ = w13.next()
        dma(P, "sp", w[:, :], io["winF"][ci - io["wf_off"], :, :], [ro], [wb_])
        pt, pb = pp.next()
        for k in range(KD):
            mm(P, pt[:, 0:Tt], w[:, k * 128:(k + 1) * 128], h3[:, k, 0:Tt], k == 0, k == KD - 1, [wb_, hb], [pb])
        return pt, pb

    NM = 256
    dma(P, "sp", xt[:, :, 0:NM], io["memT"][:, :].rearrange("p (k t) -> p k t", k=KD), [ro], [xb])
    rmsnorm_T(P, C, xt[:, :, 0:NM], xb, gT, gTb, G_MEM, hT[:, :, 0:NM], hb, scrA[:, :, 0:NM], scrb, pq.next(), rstd, rstdb, NM)
    mkT = P.sb("mkT", [128, 4, NM], BF16)
    mv = P.sb("mv", [128, 2, 512], BF16)
    memb = Buf("memkv")
    for c in range(4):
        w, wb_ = w13.next()
        dma(P, "sp", w[:, :], io["wkvF"][c, :, :], [ro], [wb_])
        pt, pb = pp.next()
        for k in range(KD):
            mm(P, pt[:, 0:NM], w[:, k * 128:(k + 1) * 128], hT[:, k, 0:NM], k == 0, k == KD - 1, [wb_, hb], [pb])
        cp(P, "dve", mkT[:, c, :], pt[:, 0:NM], [pb], [memb])
    for gq in range(2):
        w, wb_ = wT.next()
        dma(P, "sp", w[:, :], io["wkvT"][gq, :, :], [ro], [wb_])
        for mh in range(2):
            pt, pb = pp.next()
            for k in range(KD):
                mm(P, pt[:, 0:256], hT[:, k, mh * 128:(mh + 1) * 128], w[:, k * 256:(k + 1) * 256], k == 0, k == KD - 1, [wb_, hb], [pb])
            cp(P, "dve", mv[:, mh, gq * 256:(gq + 1) * 256], pt[:, 0:256], [pb], [memb])

    S = P.sb("S", [128, 4, 128], F32); Sbuf = Buf("S")
    Sb = P.sb("Sb", [128, 4, 9, 128], BF16); Sbb = Buf("Sb")
    Lc = P.sb("Lc", [128, 16], F32); Lcb = Buf("Lc")
    P.add("pool", lambda e: e.memset(S[:, :, :], 0.0), w=[Sbuf])
    P.add("pool", lambda e: e.memset(Sb[:, :, :, :], 0.0), w=[Sbb])
    P.add("pool", lambda e: e.memset(Lc[:, :], 0.0), w=[Lcb])
    qf = P.sb("qf", [128, TT], F32); qfb = Buf("qf")
    sg = P.sb("sg", [128, TT], F32); sgb = Buf("sg")
    tA = P.sb("tA", [128, TT], F32); tAb = Buf("tA")
    tB = P.sb("tB", [128, TT], F32); tBb = Buf("tB")
    Gt = P.sb("Gt", [128, TT], F32); Gtb = Buf("Gt")
    tK = P.sb("tK", [128, TT], F32); tKb = Buf("tK")
    tD = P.sb("tD", [128, TT], F32); tDb = Buf("tD")
    tE = P.sb("tE", [128, TT], F32); tEb = Buf("tE")
    GL = P.sb("GL", [128, 8], F32); GLb = Buf("GL")
    Linc = P.sb("Linc", [128, 8], F32); Lincb = Buf("Linc")
    Lpre = P.sb("Lpre", [128, 8], F32); Lpreb = Buf("Lpre")
    eGl = P.sb("eGl", [128, 4, 8], F32); eGlb = Buf("eGl")
    qDs = P.sb("qDs", [128, 4, TT], BF16); qDb = Buf("qDs")
    qsT = P.sb("qsT", [128, 4, TT], BF16); qsb = Buf("qsT")
    qhT = P.sb("qhT", [128, 4, TT], BF16); qhb = Buf("qhT")
    khT = P.sb("khT", [128, 4, TT], BF16); khb = Buf("khT")
    kdT = P.sb("kdT", [128, 4, TT], BF16); kdb = Buf("kdT")
    vB = P.sb("vB", [64, 8, 512], BF16); vBb = Buf("vB")
    kdt = P.sb("kdt", [64, 8, 4, 128], BF16); kdtb = Buf("kdt")
    olocs = P.sb("olocs", [64, 8, 512], BF16); olb = Buf("olocs")
    ATm = Rot(P, "ATm", 4, [64, 64], BF16)
    qmT = Rot(P, "qmT", 2, [128, TT], BF16)
    PT = Rot(P, "PT", 3, [128, TT], BF16)
    rden = P.sb("rden", [128, TT], F32); rdb = Buf("rden")
    omS = P.sb("omS", [128, 4, TT], BF16); omb = Buf("omS")
    qkst = Rot(P, "qkst", 3, [128, TT], BF16)
    kmacc = P.sb("kmacc", [128, 8, NBc], F32); kmb = Buf("kmacc")
    ones8 = P.sb("ones8", [128, 8], F32)
    P.add("pool", lambda e: e.memset(ones8[:, :], 1.0), w=[C.cb])

    def v3(t2):
        return t2[:, :].rearrange("p (c t) -> p c t", t=64)

    B3 = [128, 8, 64]
    ob_q = Buf("QT", multi=True); ob_k = Buf("KT", multi=True); ob_v = Buf("V", multi=True)
    ob_om = Buf("omT", multi=True); ob_ol = Buf("oloc", multi=True); ob_qd = Buf("qDT", multi=True)

    import os
    STAGE = int(os.environ.get("M1_STAGE", "99"))
    for i in range(NT):
        if STAGE < 1:
            break
        t0 = i * TT
        dma(P, "sp", xt[:, :, :], io["xmid"][i].rearrange("p (k t) -> p k t", k=KD), [ro], [xb])
        rmsnorm_T(P, C, xt, xb, gT, gTb, G_MIX, hT, hb, scrA, scrb, pq.next(), rstd, rstdb, TT)
        if STAGE < 2:
            continue
        for gq in range(2):
            w, wb_ = wT.next()
            dma(P, "sp", w[:, :], io["winT"][4 + gq, :, :], [ro], [wb_])
            for c in range(8):
                pt, pb = pp.next()
                for k in range(KD):
                    mm(P, pt[0:64, 0:256], hT[:, k, c * 64:(c + 1) * 64], w[:, k * 256:(k + 1) * 256], k == 0, k == KD - 1, [wb_, hb], [pb])
                cp(P, "act" if c % 2 else "dve", vB[:, c, gq * 256:(gq + 1) * 256], pt[0:64, 0:256], [pb], [vBb])
        for h in range(int(os.environ.get("NHG", "4"))):
            pt, pb = projF(16 + h, hT, TT)
            act(P, qf[:, :], pt[:, 0:TT], AF.Silu, [pb], [qfb])
            pt, pb = projF(20 + h, hT, TT)
            act(P, sg[:, :], pt[:, 0:TT], AF.Sigmoid, [pb], [sgb])
            ts(P, "dve", tA[:, :], sg[:, :], oml[:, h:h + 1], lb[:, h:h + 1], ALU.mult, ALU.add, [sgb, lbb], [tAb])
            ts(P, "pool", tA[:, :], tA[:, :], FMIN, None, ALU.max, None, [tAb], [tAb])
            act(P, tB[:, :], tA[:, :], AF.Ln, [tAb], [tBb])
            P.add("dve", lambda e: e.tensor_tensor_scan(out=Gt[:, :], data0=smask[:, :], data1=tB[:, :], initial=0.0,
                                                        op0=ALU.mult, op1=ALU.add), r=[tBb, cb2], w=[Gtb])
            ts(P, "pool", tK[:, :], sg[:, :], -1.0, lbm1[:, h:h + 1], ALU.add, ALU.mult, [sgb, lbb], [tKb])
            G3 = v3(Gt)
            cp(P, "pool", GL[:, :], G3[:, :, 63], [Gtb], [GLb])
            P.add("dve", lambda e, h=h: e.tensor_tensor_scan(out=Linc[:, :], data0=ones8[:, :], data1=GL[:, :], initial=Lc[:, h:h + 1],
                                                             op0=ALU.mult, op1=ALU.add), r=[GLb, Lcb, C.cb], w=[Lincb])
            cp(P, "pool", Lpre[:, 0:1], Lc[:, h:h + 1], [Lcb], [Lpreb])
            cp(P, "pool", Lpre[:, 1:8], Linc[:, 0:7], [Lincb], [Lpreb])
            cp(P, "pool", Lc[:, h:h + 1], Linc[:, 7:8], [Lincb, Lpreb], [Lcb])
            act(P, eGl[:, h, :], GL[:, :], AF.Exp, [GLb], [eGlb])
            act(P, tE[:, :], Gt[:, :], AF.Exp, [Gtb], [tEb])
            tt(P, "pool", qsT[:, h, :], qf[:, :], tE[:, :], ALU.mult, [qfb, tEb], [qsb])
            tt(P, "dve", v3(tD), G3, Lpre[:, :].unsqueeze(2).to_broadcast(B3), ALU.add, [Gtb, Lpreb], [tDb])
            act(P, tE[:, :], tD[:, :], AF.Exp, [tDb], [tEb])
            tt(P, "pool", qDs[:, h, :], qf[:, :], tE[:, :], ALU.mult, [qfb, tEb], [qDb])
            tt(P, "dve", v3(tD), G3, G3[:, :, 31:32].to_broadcast(B3), ALU.subtract, [Gtb], [tDb])
            act(P, tE[:, :], tD[:, :], AF.Exp, [tDb], [tEb])
            tt(P, "pool", qhT[:, h, :], qf[:, :], tE[:, :], ALU.mult, [qfb, tEb], [qhb])
            act(P, tE[:, :], tD[:, :], AF.Exp, [tDb], [tEb], scale=-1.0)
            tt(P, "pool", khT[:, h, :], tK[:, :], tE[:, :], ALU.mult, [tKb, tEb], [khb])
            tt(P, "dve", v3(tD), G3[:, :, 63:64].to_broadcast(B3), G3, ALU.subtract, [Gtb], [tDb])
            act(P, tE[:, :], tD[:, :], AF.Exp, [tDb], [tEb])
            tt(P, "pool", kdT[:, h, :], tK[:, :], tE[:, :], ALU.mult, [tKb, tEb], [kdb])
        if STAGE < 3:
            continue
        for h in range(8):
            pt, pb = projF(h, hT, TT)
            st, stb = qkst.next()
            cp(P, "act", st[:, :], pt[:, 0:TT], [pb], [stb])
            dma(P, "sp", io["QT"][h, :, t0:t0 + TT], st[:, :], [stb], [ob_q])
        for h in range(8):
            pt, pb = projF(8 + h, hT, TT)
            st, stb = qkst.next()
            cp(P, "act", st[:, :], pt[:, 0:TT], [pb], [stb])
            dma(P, "sp", io["KT"][h, :, t0:t0 + TT], st[:, :], [stb], [ob_k])
            P.add("dve", lambda e, pt=pt, h=h, i=i: e.reduce_sum(out=kmacc[:, h, 2 * i:2 * i + 2],
                                                                in_=pt[:, 0:TT].rearrange("p (b t) -> p b t", t=256), axis=AX.X),
                  r=[pb], w=[kmb])
        Vst = scrA[:, 0:8, :]
        for gq in range(4):
            w, wb_ = wT.next()
            dma(P, "sp", w[:, :], io["winT"][gq, :, :], [ro], [wb_])
            for tc in range(4):
                pt, pb = pp.next()
                for k in range(KD):
                    mm(P, pt[:, 0:256], hT[:, k, tc * 128:(tc + 1) * 128], w[:, k * 256:(k + 1) * 256], k == 0, k == KD - 1, [wb_, hb], [pb])
                cp(P, "act" if tc % 2 else "dve", Vst[:, 2 * tc + gq // 2, (gq % 2) * 256:(gq % 2) * 256 + 256], pt[:, 0:256], [pb], [scrb])
        dma(P, "pool", io["V"][t0:t0 + TT, :].rearrange("(c t) (a f) -> t c a f", t=128, f=512),
            Vst.rearrange("p (c a) f -> p c a f", a=2), [scrb], [ob_v])
        if STAGE < 4:
            continue
        for h in range(4):
            pt, pb = projF(24 + h, hT, TT)
            qm, qmb = qmT.next()
            cp(P, "dve", qm[:, :], pt[:, 0:TT], [pb], [qmb])
            pd, pdb = pq.next()
            po, pob = pq.next()
            for mh in range(2):
                ps_, psb = pp.next()
                mm(P, ps_[:, 0:TT], mkT[:, h, mh * 128:(mh + 1) * 128], qm[:, :], True, True, [memb, qmb], [psb])
                pT_, pTb_ = PT.next()
                act(P, pT_[:, :], ps_[:, 0:TT], AF.Exp, [psb], [pTb_], scale=SCALE)
                mm(P, pd[:, 0:TT], C.ones1[:, :], pT_[:, :], mh == 0, mh == 1, [C.cb, pTb_], [pdb])
                mm(P, po[:, 0:TT], mv[:, mh, h * 128:(h + 1) * 128], pT_[:, :], mh == 0, mh == 1, [memb, pTb_], [pob])
            P.add("dve", lambda e, pd=pd: e.reciprocal(out=rden[:, :], in_=pd[:, 0:TT]), r=[pdb], w=[rdb])
            tt(P, "dve", omS[:, h, :], po[:, 0:TT], rden[:, :], ALU.mult, [pob, rdb], [omb])
        dma(P, "pool", io["omT"][:, :, t0:t0 + TT].rearrange("h p t -> p h t"), omS[:, :, :], [omb], [ob_om])
        dma(P, "pool", io["qDT"][:, :, t0:t0 + TT].rearrange("h p t -> p h t"), qDs[:, :, :], [qDb], [ob_qd])
        if STAGE < 5:
            continue
        for c in range(8):
            ptr, ptrb = pTr.next()
            for h in range(4):
                tp(P, ptr[0:64, h * 128:(h + 1) * 128], kdT[:, h, c * 64:(c + 1) * 64], ident[:, :], [kdb, cb2], [ptrb])
            cp(P, "act" if c % 2 else "dve", kdt[:, c, :, :], ptr[0:64, 0:512].rearrange("p (h d) -> p h d", h=4), [ptrb], [kdtb])
        cp(P, "pool", Sb[:, :, 0, :], Sb[:, :, 8, :], [Sbb], [Sbb])
        for c in range(8):
            pu_, pub_ = pU.next()
            for h in range(4):
                mm(P, pu_[:, h * 128:(h + 1) * 128], kdt[:, c, h, :], vB[:, c, h * 128:(h + 1) * 128], True, True, [kdtb, vBb], [pub_])
            for h in range(4):
                stt(P, S[:, h, :], S[:, h, :], eGl[:, h, c:c + 1], pu_[:, h * 128:(h + 1) * 128], ALU.mult, ALU.add, [Sbuf, eGlb, pub_], [Sbuf])
            cp(P, "act", Sb[:, :, c + 1, :], S[:, :, :], [Sbuf], [Sbb])
        for c in range(8):
            for h in range(4):
                pa, pab = pAT.next()
                mm(P, pa[0:64, 0:64], khT[:, h, c * 64:(c + 1) * 64], qhT[:, h, c * 64:(c + 1) * 64], True, True, [khb, qhb], [pab])
                am, amb = ATm.next()
                tt(P, "dve", am[:, :], pa[0:64, 0:64], tri[:, :], ALU.mult, [pab, cb2], [amb])
                po_, pob_ = pO.next()
                mm(P, po_[0:64, 0:128], am[:, :], vB[:, c, h * 128:(h + 1) * 128], True, False, [amb, vBb], [pob_])
                mm(P, po_[0:64, 0:128], qsT[:, h, c * 64:(c + 1) * 64], Sb[:, h, c, :], False, True, [qsb, Sbb], [pob_])
                cp(P, "act", olocs[:, c, h * 128:(h + 1) * 128], po_[0:64, 0:128], [pob_], [olb])
        dma(P, "pool", io["oloc"][t0:t0 + TT, :].rearrange("(c t) f -> t c f", t=64), olocs[:, :, :], [olb], [ob_ol])
    ob_s = Buf("Sfin"); ob_l = Buf("Ltot"); ob_km = Buf("kmT")
    dma(P, "pool", io["Sfin"][:, :, :].rearrange("h p v -> p h v"), S[:, :, :], [Sbuf], [ob_s])
    dma(P, "pool", io["Ltot"][:, :], Lc[:, :], [Lcb], [ob_l])
    ts(P, "dve", kmacc[:, :, :], kmacc[:, :, :], 1.0 / 256, None, ALU.mult, None, [kmb], [kmb])
    dma(P, "pool", io["kmT"][:, :], kmacc[:, :, :].rearrange("p h b -> p (h b)"), [kmb], [ob_km])
    end_phase(P, nc)


def phase_a(nc, T, io):
    P = Prog(nc)
    C = Ctx()
    setup_common(P, C)
    ro = C.ro
    NB = T // 256
    NS = T // 128
    ident = P.sb("ident", [128, 128], BF16)
    Esel = P.sb("Esel", [68, NB * 128], BF16)
    CM = P.sb("CM", [128, 512], BF16)
    cb2 = Buf("c2")
    dma(P, "sp", ident[:, :], io["ident"][:, :], [ro], [cb2])
    dma(P, "sp", Esel[:, :], io["Esel"][:, :], [ro], [cb2])
    dma(P, "sp", CM[:, :], io["CM"][:, :], [ro], [cb2])
    QT = P.sb("QT", [128, T], BF16); QTb = Buf("QT", multi=True)
    KT = P.sb("KT", [128, T], BF16); KTb = Buf("KT", multi=True)
    Va = P.sb("Va", [128, NS, 129], BF16); Vab = Buf("Va", multi=True)
    km32 = P.sb("km32", [128, NB], F32); kmb = Buf("km32")
    kmhi = P.sb("kmhi", [128, NB], BF16)
    kmlo = P.sb("kmlo", [128, NB], BF16)
    kmt = P.sb("kmt", [128, NB], F32)
    kmhb = Buf("kmhl")
    btab = P.sb("btab", [128, NB], F32); btb = Buf("btab")
    MT = P.sb("MT", [68, 512], BF16); MTb = Buf("MT")
    gs = P.sb("gs", [128, 2, 64], F32); gsb = Buf("gs")
    mx = P.sb("mx", [128, 2, 8], F32); mxb = Buf("mx")
    nm = P.sb("nm", [128, 2, 64], BF16); nmb = Buf("nm")
    rec = P.sb("rec", [128, 2], F32); recb = Buf("rec")
    on = P.sb("on", [128, 2, 128], BF16); onb = Buf("on")
    oTs = Rot(P, "oTs", 2, [128, 256], BF16)
    pT = Rot(P, "pT", 3, [128, 512], BF16)
    ps = Rot(P, "ps", 3, [128, 512], F32, psum=True)
    po = Rot(P, "po", 2, [128, 512], F32, psum=True)
    pg = Rot(P, "pg", 1, [128, 512], F32, psum=True)
    ptr = Rot(P, "ptr", 1, [128, 1024], BF16, psum=True)
    ob = Buf("oaT", multi=True)
    NQ = 4
    for hh in range(2):
        for q in range(NQ):
            a, b_ = q * T // NQ, (q + 1) * T // NQ
            dma(P, "sp", QT[:, a:b_], io["QTf"][hh, :, a:b_], [ro], [QTb])
            dma(P, "sp", KT[:, a:b_], io["KTf"][hh, :, a:b_], [ro], [KTb])
            sa, sb_ = q * NS // NQ, (q + 1) * NS // NQ
            dma(P, "sp", Va[:, sa:sb_, :], io["Vf"][hh, :, sa * 129:sb_ * 129].rearrange("p (c f) -> p c f", f=129), [ro], [Vab])
        dma(P, "sp", km32[:, :], io["kmf"][hh, :, :], [ro], [kmb])
        dma(P, "sp", btab[:, :], io["btab"][hh, :, :], [ro], [btb])
        dma(P, "sp", MT[64:68, :], io["qrow"][hh, :, :], [ro], [MTb])
        cp(P, "dve", kmhi[:, :], km32[:, :], [kmb], [kmhb])
        cp(P, "dve", kmt[:, :], kmhi[:, :], [kmhb], [kmhb])
        tt(P, "dve", kmt[:, :], km32[:, :], kmt[:, :], ALU.subtract, [kmb, kmhb], [kmhb])
        cp(P, "dve", kmlo[:, :], kmt[:, :], [kmhb], [kmhb])
        P.add("pool", lambda e: e.memset(gs[:, :, :], -1e30), w=[gsb])
        for B in range(NB):
            q0 = B * 256
            if B >= 1:
                g_, gb_ = pg.next()
                g3 = g_[:, 0:128].rearrange("p (c b) -> p c b", c=2)
                for qc in range(2):
                    mm(P, g3[:, qc, 0:B], QT[:, q0 + qc * 128:q0 + (qc + 1) * 128], kmhi[:, 0:B], True, False, [QTb, kmhb], [gb_])
                    mm(P, g3[:, qc, 0:B], QT[:, q0 + qc * 128:q0 + (qc + 1) * 128], kmlo[:, 0:B], False, True, [QTb, kmhb], [gb_])
                cp(P, "dve", gs[:, :, 0:B], g3[:, :, 0:B], [gb_], [gsb])
                for qc in range(2):
                    P.add("dve", lambda e, qc=qc: e.max(out=mx[:, qc, :], in_=gs[:, qc, :]), r=[gsb], w=[mxb])
                for qc in range(2):
                    ts(P, "dve", nm[:, qc, :], gs[:, qc, :], mx[:, qc, 2:3], -NEG, ALU.is_ge, ALU.mult, [gsb, mxb], [nmb])
                ts(P, "pool", nm[:, :, :], nm[:, :, :], NEG, None, ALU.add, None, [nmb], [nmb])
                tr_, trb = ptr.next()
                for qc in range(2):
                    tp(P, tr_[0:64, qc * 128:(qc + 1) * 128], nm[:, qc, :], ident[:, :], [nmb, cb2], [trb])
                cp(P, "act", MT[0:64, 0:256], tr_[0:64, 0:256], [trb], [MTb])
                cp(P, "dve", MT[0:64, 256:512], tr_[0:64, 0:256], [trb], [MTb])
            o_, ob_ = po.next()
            o3 = o_[:, 0:260].rearrange("p (c f) -> p c f", c=2)
            nmm = 0
            total = (B + 1) * 4
            for b in range(B + 1):
                s_, sb2 = ps.next()
                qsl = QT[:, q0:q0 + 256]
                mm(P, s_[:, 0:256], KT[:, (2 * b) * 128:(2 * b + 1) * 128], qsl, True, False, [KTb, QTb], [sb2])
                mm(P, s_[:, 256:512], KT[:, (2 * b + 1) * 128:(2 * b + 2) * 128], qsl, False, False, [KTb, QTb], [sb2], skip_group_check=True)
                if b == B:
                    mm(P, s_[:, 0:512], Esel[64:68, 0:128], MT[64:68, :], False, False, [cb2, MTb], [sb2], skip_group_check=True)
                    mm(P, s_[:, 0:512], ident[:, :], CM[:, :], False, True, [cb2], [sb2], skip_group_check=True)
                else:
                    mm(P, s_[:, 0:512], Esel[0:68, b * 128:(b + 1) * 128], MT[0:68, :], False, True, [cb2, MTb], [sb2], skip_group_check=True)
                p_, pb_ = pT.next()
                act(P, p_[:, :], s_[:, 0:512], AF.Exp, [sb2, btb], [pb_], bias=btab[:, B - b:B - b + 1], scale=SCALE)
                for j in range(2):
                    for qc in range(2):
                        nmm += 1
                        mm(P, o3[:, qc, 0:129], p_[:, j * 256 + qc * 128:j * 256 + (qc + 1) * 128], Va[:, 2 * b + j, :],
                           nmm == 1, nmm > total - 2, [pb_, Vab], [ob_], skip_group_check=True)
            P.add("dve", lambda e, o3=o3: e.reciprocal(out=rec[:, :], in_=o3[:, :, 128]), r=[ob_], w=[recb])
            for qc in range(2):
                ts(P, "dve", on[:, qc, :], o3[:, qc, 0:128], rec[:, qc:qc + 1], None, ALU.mult, None, [ob_, recb], [onb])
            tr_, trb = ptr.next()
            for qc in range(2):
                tp(P, tr_[:, 512 + qc * 128:512 + (qc + 1) * 128], on[:, qc, :], ident[:, :], [onb, cb2], [trb])
            ot, otb = oTs.next()
            cp(P, "act", ot[:, :], tr_[:, 512:768], [trb], [otb])
            dma(P, "pool", io["oaT"][hh, :, q0:q0 + 256], ot[:, :], [otb], [ob])
    end_phase(P, nc)


def phase_m3(nc, NT, io):
    P = Prog(nc)
    C = Ctx()
    setup_common(P, C)
    ro = C.ro
    gT = P.sb("gT", [128, 5 * KD], F32); gTb = Buf("gT")
    dma(P, "sp", gT[:, :], io["gains"][:, :], [ro], [gTb])
    ident = P.sb("ident", [128, 128], BF16)
    ogn = P.sb("ogn", [128, 128], F32)
    rsel = P.sb("rsel", [128, 16], F32)
    cb2 = Buf("c2")
    dma(P, "sp", ident[:, :], io["ident"][:, :], [ro], [cb2])
    dma(P, "sp", ogn[:, :], io["ogn"][:, :], [ro], [cb2])
    dma(P, "sp", rsel[:, :], io["rsel"][:, :], [ro], [cb2])
    St = P.sb("St", [128, 4, 128], F32); Stb = Buf("St")
    Sin = P.sb("Sin", [128, 4, 128], F32); Sinb_ = Buf("Sin")
    Sf = P.sb("Sf", [128, 4, 128], F32); Sfb = Buf("Sf")
    Lt = P.sb("Lt", [128, 16], F32); Ltb = Buf("Lt")
    Sinh = P.sb("Sinh", [128, 4, 128], BF16); Sinhb = Buf("Sinh")
    P.add("pool", lambda e: e.memset(St[:, :, :], 0.0), w=[Stb])
    P.add("pool", lambda e: e.memset(Sin[:, :, :], 0.0), w=[Sinb_])
    for rp in range(3):
        dma(P, "sp", Sf[:, :, :], io["SfinAll"][rp].rearrange("h p v -> p h v"), [ro], [Sfb])
        dma(P, "sp", Lt[:, :], io["LtotAll"][rp], [ro], [Ltb])
        act(P, Lt[:, :], Lt[:, :], AF.Exp, [Ltb], [Ltb])
        for h in range(4):
            stt(P, St[:, h, :], St[:, h, :], Lt[:, h:h + 1], Sf[:, h, :], ALU.mult, ALU.add, [Stb, Ltb, Sfb], [Stb])
        stt(P, Sin[:, :, :], St[:, :, :], rsel[:, rp:rp + 1], Sin[:, :, :], ALU.mult, ALU.add, [Stb, cb2, Sinb_], [Sinb_])
    cp(P, "dve", Sinh[:, :, :], Sin[:, :, :], [Sinb_], [Sinhb])

    xt = P.sb("xt", [128, KD, TT], F32); xb = Buf("xt")
    hT = P.sb("hT", [128, KD, TT], BF16); hb = Buf("hT")
    yT = P.sb("yT", [128, KD, TT], BF16); yb = Buf("yT")
    rstd = P.sb("rstd", [128, TT], F32); rstdb = Buf("rstd")
    w13 = Rot(P, "w13", 3, [128, KD * 128], BF16)
    wp = Rot(P, "wp", 2, [128, 2048], BF16)
    qD = P.sb("qD", [128, 4, TT], BF16); qDb = Buf("qD")
    olt = P.sb("olt", [128, 4, 512], BF16); oltb = Buf("olt")
    oaS = P.sb("oaS", [128, 8, TT], BF16); oab = Buf("oaS")
    omS = P.sb("omS", [128, 4, TT], BF16); omb = Buf("omS")
    sgb = P.sb("sgb", [128, 4, TT], BF16); sgbb = Buf("sgb")
    obg = P.sb("obg", [128, 4, TT], BF16); obgb = Buf("obg")
    of = P.sb("of", [128, 4, 128], F32); ofb = Buf("of")
    junk = P.sb("junk", [128, 4, 128], F32); jb = Buf("junk")
    ss = P.sb("ss", [128, 4], F32); ssb = Buf("ss")
    onrm = P.sb("onrm", [128, 4, 128], BF16); onb = Buf("onrm")
    sgt = Rot(P, "sgt", 2, [128, TT], BF16)
    acc = P.sb("acc", [128, TT], F32); accb = Buf("acc")
    tmp = Rot(P, "tmp", 2, [128, TT], F32)
    pp = Rot(P, "pp", 2, [128, 512], F32, psum=True)
    pq = Rot(P, "pq", 2, [128, 512], F32, psum=True)
    pss = Rot(P, "pss", 1, [128, 512], F32, psum=True)
    pc = Rot(P, "pc", 1, [128, 512], F32, psum=True)
    ptr = Rot(P, "ptr", 1, [128, 1024], BF16, psum=True)
    outb = Buf("x2", multi=True)

    def projF(ci):
        w, wb_ = w13.next()
        dma(P, "sp", w[:, :], io["winF"][ci - io["wf_off"], :, :], [ro], [wb_])
        pt, pb = pp.next()
        for k in range(KD):
            mm(P, pt[:, 0:TT], w[:, k * 128:(k + 1) * 128], hT[:, k, :], k == 0, k == KD - 1, [wb_, hb], [pb])
        return pt, pb

    for i in range(NT):
        t0 = i * TT
        dma(P, "sp", xt[:, :, :], io["xmid"][i].rearrange("p (k t) -> p k t", k=KD), [ro], [xb])
        dma(P, "sp", qD[:, :, :], io["qDT"][:, :, t0:t0 + TT].rearrange("h p t -> p h t"), [ro], [qDb])
        dma(P, "sp", olt[:, :, :], io["oloc"][t0:t0 + TT, :].rearrange("(c t) f -> t c f", t=128), [ro], [oltb])
        dma(P, "sp", oaS[:, :, :], io["oaTc"][:, :, t0:t0 + TT].rearrange("h p t -> p h t"), [ro], [oab])
        dma(P, "sp", omS[:, :, :], io["omT"][:, :, t0:t0 + TT].rearrange("h p t -> p h t"), [ro], [omb])
        rmsnorm_T(P, C, xt, xb, gT, gTb, G_MIX, hT, hb, yT, yb, pss.next(), rstd, rstdb, TT)
        for h in range(4):
            pt, pb = projF(28 + h)
            act(P, sgb[:, h, :], pt[:, 0:TT], AF.Sigmoid, [pb], [sgbb])
        for tc in range(4):
            pc_, pcb = pc.next()
            for h in range(4):
                mm(P, pc_[:, h * 128:(h + 1) * 128], qD[:, h, tc * 128:(tc + 1) * 128], Sinh[:, h, :], True, True, [qDb, Sinhb], [pcb])
            tt(P, "dve", of[:, :, :], pc_[:, 0:512].rearrange("p (h v) -> p h v", h=4), olt[:, tc, :].rearrange("p (h v) -> p h v", h=4),
               ALU.add, [pcb, oltb], [ofb])
            act(P, junk[:, :, :], of[:, :, :], AF.Square, [ofb], [jb])
            P.add("dve", lambda e: e.reduce_sum(out=ss[:, :], in_=junk[:, :, :], axis=AX.X), r=[jb], w=[ssb])
            ts(P, "dve", ss[:, :], ss[:, :], 1.0 / 128, EPS, ALU.mult, ALU.add, [ssb], [ssb])
            act(P, ss[:, :], ss[:, :], AF.Sqrt, [ssb], [ssb])
            P.add("dve", lambda e: e.reciprocal(out=ss[:, :], in_=ss[:, :]), r=[ssb], w=[ssb])
            for h in range(4):
                stt(P, onrm[:, h, :], of[:, h, :], ss[:, h:h + 1], ogn[:, :], ALU.mult, ALU.mult, [ofb, ssb, cb2], [onb])
            tr_, trb = ptr.next()
            for h in range(4):
                tp(P, tr_[:, h * 128:(h + 1) * 128], onrm[:, h, :], ident[:, :], [onb, cb2], [trb])
            tt(P, "dve", obg[:, :, tc * 128:(tc + 1) * 128], tr_[:, 0:512].rearrange("p (h t) -> p h t", h=4),
               sgb[:, :, tc * 128:(tc + 1) * 128], ALU.mult, [trb, sgbb], [obgb])
        srcs = [(oaS, oab, 8, 0), (obg, obgb, 4, 1024), (omS, omb, 4, 1536)]
        for o in range(KD):
            w_, wpb = wp.next()
            dma(P, "sp", w_[:, :], io["wproj"][o, :, :], [ro], [wpb])
            for br in range(3):
                pt, pb = projF(32 + br * 16 + o)
                sg_, sgtb = sgt.next()
                act(P, sg_[:, :], pt[:, 0:TT], AF.Sigmoid, [pb], [sgtb])
                src, srcb, nk, off = srcs[br]
                py, pyb = pq.next()
                for k in range(nk):
                    mm(P, py[:, 0:TT], w_[:, off + k * 128:off + (k + 1) * 128], src[:, k, :], k == 0, k == nk - 1, [wpb, srcb], [pyb])
                if br == 0:
                    tt(P, "dve", acc[:, :], py[:, 0:TT], sg_[:, :], ALU.mult, [pyb, sgtb], [accb])
                else:
                    tm, tmb = tmp.next()
                    tt(P, "dve", tm[:, :], py[:, 0:TT], sg_[:, :], ALU.mult, [pyb, sgtb], [tmb])
                    if br == 1:
                        tt(P, "pool", acc[:, :], acc[:, :], tm[:, :], ALU.add, [accb, tmb], [accb])
                    else:
                        tt(P, "pool", yT[:, o, :], acc[:, :], tm[:, :], ALU.add, [accb, tmb], [yb])
        for o2 in range(KD):
            w, wb_ = w13.next()
            dma(P, "sp", w[:, :], io["wout"][o2, :, :], [ro], [wb_])
            py, pyb = pq.next()
            for k in range(KD):
                mm(P, py[:, 0:TT], w[:, k * 128:(k + 1) * 128], yT[:, k, :], k == 0, k == KD - 1, [wb_, yb], [pyb])
            tt(P, "dve", xt[:, o2, :], xt[:, o2, :], py[:, 0:TT], ALU.add, [xb, pyb], [xb])
        dma(P, "pool", io["x2"][i].rearrange("p (k t) -> p k t", k=KD), xt[:, :, :], [xb], [outb])
    end_phase(P, nc)


import ml_dtypes

NBF16 = ml_dtypes.bfloat16
NCORES = 8
LAUNCH_LOG = []
DEBUG = None


def tile_w(w):
    K, N = w.shape
    return np.ascontiguousarray(w.reshape(K // 128, 128, N // 128, 128).transpose(2, 1, 0, 3)).reshape(N // 128, 128, K)


def tile_wT(w, cols):
    K, N = w.shape
    return np.ascontiguousarray(w.reshape(K // 128, 128, N // cols, cols).transpose(2, 1, 0, 3)).reshape(N // cols, 128, (K // 128) * cols)


def fm(v):
    return np.ascontiguousarray(v.reshape(KD, 128).T)


def _dram(nc, name, shape, dt, kind):
    return nc.dram_tensor(name, list(shape), dt, kind=kind).ap()


def build_A(NT):
    nc = bass.Bass("TRN2", target_bir_lowering=False)
    TC = NT * TT
    I = lambda n, s, d=F32: _dram(nc, n, s, d, "ExternalInput")
    O = lambda n, s, d=F32: _dram(nc, n, s, d, "ExternalOutput")
    S = lambda n, s, d=BF16: _dram(nc, n, s, d, "Internal")
    xin = I("xin", [NT, 128, KD * TT])
    gains = I("gains", [128, 5 * KD])
    wsh = {"f1w1": [NF, 128, 2048], "f1w3": [NF, 128, 2048], "f1w2": [KD, 128, DFF], "winF": [28, 128, 2048],
           "winT": [6, 128, KD * 256], "wkvF": [4, 128, 2048], "wkvT": [2, 128, KD * 256]}
    w32 = {k: I(k, v) for k, v in wsh.items()}
    w16 = {k: S(k + "_b", v) for k, v in wsh.items()}
    io = {"gains": gains, "memT": I("memT", [128, KD * 256]), "lbT": I("lbT", [128, 16]), "lsel": I("lsel", [128, 16]),
          "ident": I("ident", [128, 128], BF16), "tri64": I("tri64", [64, 64], BF16), "scanmask": I("scanmask", [128, TT]),
          "winF": w16["winF"], "wf_off": 0, "winT": w16["winT"], "wkvF": w16["wkvF"], "wkvT": w16["wkvT"]}
    io["xmid"] = O("xmid", [NT, 128, KD * TT])
    io["QT"] = O("QT", [8, 128, TC], BF16)
    io["KT"] = O("KT", [8, 128, TC], BF16)
    io["V"] = O("V", [TC, 1024], BF16)
    io["kmT"] = O("kmT", [128, 8 * (TC // 256)])
    io["omT"] = O("omT", [4, 128, TC], BF16)
    io["oloc"] = O("oloc", [TC, 512], BF16)
    io["qDT"] = O("qDT", [4, 128, TC], BF16)
    io["Sfin"] = O("Sfin", [4, 128, 128])
    io["Ltot"] = O("Ltot", [128, 16])
    import os
    if os.environ.get("SKIP_CAST") != "1":
        phase_cast(nc, [(w32[k], w16[k], v[0], v[2]) for k, v in wsh.items()])
    if os.environ.get("SKIP_FFN") != "1":
        phase_ffn(nc, NT, xin, io["xmid"], gains, G_FFN1, w16["f1w1"], w16["f1w3"], w16["f1w2"])
    if os.environ.get("SKIP_M1") != "1":
        phase_m1(nc, NT, io)
    return nc


def build_B(T):
    nc = bass.Bass("TRN2", target_bir_lowering=False)
    NB, NS = T // 256, T // 128
    I = lambda n, s, d=F32: _dram(nc, n, s, d, "ExternalInput")
    io = {"ident": I("ident", [128, 128], BF16), "Esel": I("Esel", [68, NB * 128], BF16), "CM": I("CM", [128, 512], BF16),
          "QTf": I("QTf", [2, 128, T], BF16), "KTf": I("KTf", [2, 128, T], BF16), "Vf": I("Vf", [2, 128, NS * 129], BF16),
          "kmf": I("kmf", [2, 128, NB]), "btab": I("btab", [2, 128, NB]), "qrow": I("qrow", [2, 4, 512], BF16)}
    io["oaT"] = _dram(nc, "oaT", [2, 128, T], BF16, "ExternalOutput")
    phase_a(nc, T, io)
    return nc


def build_C(NT, final):
    nc = bass.Bass("TRN2", target_bir_lowering=False)
    TC = NT * TT
    I = lambda n, s, d=F32: _dram(nc, n, s, d, "ExternalInput")
    S = lambda n, s, d=BF16: _dram(nc, n, s, d, "Internal")
    gains = I("gains", [128, 5 * KD])
    wsh = {"winF": [52, 128, 2048], "wproj": [KD, 128, 2048], "wout": [KD, 128, 2048],
           "f2w1": [NF, 128, 2048], "f2w3": [NF, 128, 2048], "f2w2": [KD, 128, DFF]}
    w32 = {k: I(k, v) for k, v in wsh.items()}
    w16 = {k: S(k + "_b", v) for k, v in wsh.items()}
    io = {"gains": gains, "ident": I("ident", [128, 128], BF16), "ogn": I("ogn", [128, 128]), "rsel": I("rsel", [128, 16]),
          "xmid": I("xmid", [NT, 128, KD * TT]), "oaTc": I("oaTc", [8, 128, TC], BF16), "omT": I("omT", [4, 128, TC], BF16),
          "oloc": I("oloc", [TC, 512], BF16), "qDT": I("qDT", [4, 128, TC], BF16),
          "SfinAll": I("SfinAll", [4, 4, 128, 128]), "LtotAll": I("LtotAll", [4, 128, 16]),
          "winF": w16["winF"], "wf_off": 28, "wproj": w16["wproj"], "wout": w16["wout"]}
    io["x2"] = _dram(nc, "x2", [NT, 128, KD * TT], F32, "Internal")
    xout = _dram(nc, "xout", [NT, 128, KD * TT], F32, "ExternalOutput")
    phase_cast(nc, [(w32[k], w16[k], v[0], v[2]) for k, v in wsh.items()])
    phase_m3(nc, NT, io)
    phase_ffn(nc, NT, io["x2"], xout, gains, G_FFN2, w16["f2w1"], w16["f2w3"], w16["f2w2"],
              final_gcol=(G_FIN if final else None))
    return nc


def _run(nc, in_maps):
    res = run_bass_kernel_spmd(nc, in_maps, core_ids=list(range(NCORES)))
    LAUNCH_LOG.append(getattr(res, "exec_time_ns", None))
    return res.results


def _hilo(v):
    hi = v.astype(NBF16)
    lo = (v - hi.astype(np.float32)).astype(NBF16)
    return hi, lo


def moba_consts(T):
    NB = T // 256
    slopes = np.exp2(-8.0 * np.arange(1, 9, dtype=np.float32) / 8).astype(np.float32)
    Esel = np.zeros((68, NB, 128), np.float32)
    for b in range(NB):
        Esel[b, b, :] = 1.0
    Esel[64:68] = 1.0
    s = np.arange(128)[:, None]
    col = np.arange(512)[None, :]
    j, qi = col // 256, col % 256
    CM = np.where(j * 128 + s <= qi, 0.0, NEG).astype(np.float32)
    btab = np.zeros((8, 128, NB), np.float32)
    qrow = np.zeros((8, 4, 512), NBF16)
    for h in range(8):
        m = float(slopes[h])
        btab[h] = m * (np.arange(128, dtype=np.float32)[:, None] - 256.0 * np.arange(NB, dtype=np.float32)[None, :])
        v = (-m * (np.arange(512) % 256) / SCALE).astype(np.float32)
        hi, lo = _hilo(v)
        qrow[h, 0], qrow[h, 1] = hi, lo
        v2 = np.where(np.arange(512) >= 256, m * 128.0 / SCALE, 0.0).astype(np.float32)
        hi, lo = _hilo(v2)
        qrow[h, 2], qrow[h, 3] = hi, lo
    return Esel.reshape(68, NB * 128).astype(NBF16), CM.astype(NBF16), btab, qrow


def run_module(inp, NT):
    TC = NT * TT
    T = 4 * TC
    NB, NS = T // 256, T // 128
    x = np.asarray(inp["x"], np.float32)
    mem = np.asarray(inp["mem"], np.float32)
    ident = np.eye(128, dtype=np.float32).astype(NBF16)
    tri64 = np.triu(np.ones((64, 64), np.float32)).astype(NBF16)
    scanmask = np.ones((128, TT), np.float32)
    scanmask[:, ::64] = 0.0
    Esel, CM, btab, qrow = moba_consts(T)
    ncA = build_A(NT)
    ncB = build_B(T)
    cores = [(c // 4, c % 4) for c in range(NCORES)]
    xcur = []
    for (b, r) in cores:
        xs = x[b, r * TC:(r + 1) * TC]
        xcur.append(np.ascontiguousarray(xs.reshape(NT, TT, KD, 128).transpose(0, 3, 2, 1)).reshape(NT, 128, KD * TT))
    memT = [np.ascontiguousarray(mem[b].reshape(256, KD, 128).transpose(2, 1, 0)).reshape(128, KD * 256) for b in range(2)]
    lbT = np.zeros((128, 16), np.float32)
    lbT[:, 0:8] = np.asarray(inp["hgrn_lb_logits"], np.float32).reshape(2, 4, 128).transpose(2, 0, 1).reshape(128, 8)
    for l in range(2):
        gains = np.concatenate([fm(np.asarray(inp[k][l], np.float32)) for k in ("ffn1_norm", "mix_norm", "ffn2_norm", "mem_norm")]
                               + [fm(np.asarray(inp["final_norm"], np.float32))], axis=1)
        w_in = np.asarray(inp["w_in"][l], np.float32)
        wkv = np.asarray(inp["w_mem_kv"][l], np.float32)
        colsF = np.concatenate([np.arange(0, 2048), np.arange(3072, 4096), np.arange(5120, 5632)])
        wA = {"f1w1": tile_w(np.asarray(inp["ffn1_w1"][l], np.float32)), "f1w3": tile_w(np.asarray(inp["ffn1_w3"][l], np.float32)),
              "f1w2": tile_w(np.asarray(inp["ffn1_w2"][l], np.float32)),
              "winF": tile_w(w_in[:, colsF]),
              "winT": tile_wT(w_in[:, np.concatenate([np.arange(2048, 3072), np.arange(4096, 4608)])], 256),
              "wkvF": tile_w(wkv[:, 0:512]), "wkvT": tile_wT(wkv[:, 512:1024], 256)}
        lsel = np.full((128, 16), float(l), np.float32)
        in_maps = []
        for c, (b, r) in enumerate(cores):
            d = {"xin": xcur[c], "gains": gains, "memT": memT[b], "lbT": lbT, "lsel": lsel, "ident": ident, "tri64": tri64,
                 "scanmask": scanmask}
            d.update(wA)
            in_maps.append(d)
        resA = _run(ncA, in_maps)
        del wA, in_maps
        in_maps = []
        for c in range(NCORES):
            b, jh = c // 4, c % 4
            heads = [2 * jh, 2 * jh + 1]
            QTf = np.stack([np.concatenate([resA[b * 4 + r]["QT"][h] for r in range(4)], axis=1) for h in heads])
            KTf = np.stack([np.concatenate([resA[b * 4 + r]["KT"][h] for r in range(4)], axis=1) for h in heads])
            Vfull = np.concatenate([resA[b * 4 + r]["V"] for r in range(4)], axis=0)
            Vf = []
            for h in heads:
                vh = Vfull[:, h * 128:(h + 1) * 128].reshape(NS, 128, 128).transpose(1, 0, 2)
                va = np.ones((128, NS, 129), NBF16)
                va[:, :, 0:128] = vh
                Vf.append(va.reshape(128, NS * 129))
            kmf = np.stack([np.concatenate([resA[b * 4 + r]["kmT"].reshape(128, 8, NB // 4)[:, h] for r in range(4)], axis=1) for h in heads])
            in_maps.append({"ident": ident, "Esel": Esel, "CM": CM, "QTf": np.ascontiguousarray(QTf), "KTf": np.ascontiguousarray(KTf),
                            "Vf": np.stack(Vf), "kmf": np.ascontiguousarray(kmf), "btab": np.ascontiguousarray(btab[heads]),
                            "qrow": np.ascontiguousarray(qrow[heads])})
        resB = _run(ncB, in_maps)
        del in_maps
        wproj = np.concatenate([tile_w(np.asarray(inp["w_proj_moba"][l], np.float32)), tile_w(np.asarray(inp["w_proj_hgrn"][l], np.float32)),
                                tile_w(np.asarray(inp["w_proj_mem"][l], np.float32))], axis=2)
        wC = {"winF": tile_w(w_in[:, 4608:5120]), "wproj": np.ascontiguousarray(wproj), "wout": tile_w(np.asarray(inp["w_out"][l], np.float32)),
              "f2w1": tile_w(np.asarray(inp["ffn2_w1"][l], np.float32)), "f2w3": tile_w(np.asarray(inp["ffn2_w3"][l], np.float32)),
              "f2w2": tile_w(np.asarray(inp["ffn2_w2"][l], np.float32))}
        wC["winF"] = np.concatenate([wC["winF"], tile_w(w_in[:, 5632:11776])], axis=0)
        ogn = np.ascontiguousarray(np.broadcast_to(np.asarray(inp["hgrn_out_norm"][l], np.float32)[None, :], (128, 128)))
        ncC = build_C(NT, final=(l == 1))
        in_maps = []
        for c, (b, r) in enumerate(cores):
            oaTc = np.concatenate([resB[b * 4 + jh]["oaT"][:, :, r * TC:(r + 1) * TC] for jh in range(4)], axis=0)
            rsel = np.zeros((128, 16), np.float32)
            if r >= 1:
                rsel[:, r - 1] = 1.0
            d = {"gains": gains, "ident": ident, "ogn": ogn, "rsel": rsel, "xmid": resA[c]["xmid"], "oaTc": np.ascontiguousarray(oaTc),
                 "omT": resA[c]["omT"], "oloc": resA[c]["oloc"], "qDT": resA[c]["qDT"],
                 "SfinAll": np.stack([resA[b * 4 + rr]["Sfin"] for rr in range(4)]),
                 "LtotAll": np.stack([resA[b * 4 + rr]["Ltot"] for rr in range(4)])}
            d.update(wC)
            in_maps.append(d)
        resC = _run(ncC, in_maps)
        del wC, in_maps
        xcur = [resC[c]["xout"] for c in range(NCORES)]
        if DEBUG is not None:
            DEBUG[l] = (resA, resB, xcur)
    out = np.zeros((2, T, D), np.float32)
    for c, (b, r) in enumerate(cores):
        out[b, r * TC:(r + 1) * TC] = xcur[c].reshape(NT, 128, KD, TT).transpose(0, 3, 2, 1).reshape(TC, D)
    return out


def kernel(**inputs):
    return run_module(inputs, 8)
```
